# Optimizing a Trainium2 kernel written in Bass

```python
import math
import jax, jax.numpy as jnp
from jax import lax
import numpy as np


D_MODEL = 1024
BATCH = 32
SEQ = 2048
DEPTH = 4
DEC_BATCH = 4
DEC_SEQ = 4096
PAST_LEN = 128

HEAD_DIM = 64
N_HEADS = D_MODEL // HEAD_DIM
N_HEADS_NA = N_HEADS // 4
N_HEADS_DIL = N_HEADS - N_HEADS_NA
D_NA = N_HEADS_NA * HEAD_DIM
D_DIL = N_HEADS_DIL * HEAD_DIM
GRID_W = 64
NA_ROWS = 8
NA_COLS = 16
NA_Q_COLS = 16
NA_K_COLS = NA_Q_COLS + NA_COLS
DIL_PATTERNS = ((128, 1), (512, 4), (2048, 16))
DIL_BLOCK = 64
N_EXPERTS = 16
EXPERT_FF = D_MODEL
CAPACITY_FACTOR = 2
RMS_EPS = 1e-6

kernel_name = 'hybrid_na_dilated_ec_encoder'


def rmsnorm(x, gain):
    xf = x.astype(jnp.float32)
    y = xf * lax.rsqrt(jnp.mean(xf * xf, axis=-1, keepdims=True) + RMS_EPS)
    return (y * gain.astype(jnp.float32)).astype(x.dtype)


def alibi_slopes(n):
    def pow2(m):
        start = 2.0 ** (-8.0 / m)
        return [start ** (i + 1) for i in range(m)]
    if math.log2(n).is_integer():
        s = pow2(n)
    else:
        c = 2 ** int(math.floor(math.log2(n)))
        s = pow2(c) + pow2(2 * c)[0::2][: n - c]
    return np.asarray(s, dtype=np.float32)


def neighborhood_attention(q, k, v, rpb):
    b, s, h, dh = q.shape
    rows = s // GRID_W
    kh = min(NA_ROWS, rows)
    ncb = GRID_W // NA_Q_COLS
    r = np.arange(rows)
    key_rows = np.clip(r - NA_ROWS // 2, 0, rows - kh)[:, None] + np.arange(kh)[None, :]
    row_off = key_rows - r[:, None]
    q_cols = np.arange(ncb)[:, None] * NA_Q_COLS + np.arange(NA_Q_COLS)[None, :]
    key_col0 = np.clip(np.arange(ncb) * NA_Q_COLS - NA_COLS // 2, 0, GRID_W - NA_K_COLS)
    key_cols = key_col0[:, None] + np.arange(NA_K_COLS)[None, :]
    win0 = np.clip(q_cols - NA_COLS // 2, 0, GRID_W - NA_COLS)
    in_win = (key_cols[:, None, :] >= win0[:, :, None]) & (key_cols[:, None, :] < win0[:, :, None] + NA_COLS)
    col_off = key_cols[:, None, :] - q_cols[:, :, None]
    ri = (row_off + NA_ROWS - 1)[:, None, None, :, None]
    ci = np.clip(col_off + NA_COLS - 1, 0, 2 * NA_COLS - 2)[None, :, :, None, :]
    bias = rpb.astype(jnp.float32)[:, ri, ci]
    qg = q.reshape(b, rows, ncb, NA_Q_COLS, h, dh)

    def gather(t):
        t = jnp.take(t.reshape(b, rows, GRID_W, h, dh), key_cols, axis=2)
        return jnp.take(t, key_rows, axis=1)

    kb, vb = gather(k), gather(v)
    sc = jnp.einsum('brcqhd,brkcjhd->bhrcqkj', qg, kb, preferred_element_type=jnp.float32) * (dh ** -0.5)
    sc = jnp.where(in_win[None, :, :, None, :], sc + bias, -jnp.inf)
    p = jax.nn.softmax(sc.reshape(b, h, rows, ncb, NA_Q_COLS, kh * NA_K_COLS), axis=-1)
    p = p.reshape(sc.shape).astype(v.dtype)
    o = jnp.einsum('bhrcqkj,brkcjhd->brcqhd', p, vb)
    return o.reshape(b, s, h, dh)


def dilated_branch(q, k, v, slopes, window, dilation):
    b, s, h, dh = q.shape
    L = s // dilation
    half = window // (2 * dilation)
    n = b * dilation
    nb = -(-L // DIL_BLOCK)
    lp = nb * DIL_BLOCK
    kw = DIL_BLOCK + 2 * half

    def to_res(t):
        return jnp.swapaxes(t.reshape(b, L, dilation, h, dh), 1, 2).reshape(n, L, h, dh)

    qr, kr, vr = to_res(q), to_res(k), to_res(v)
    qb = jnp.pad(qr, ((0, 0), (0, lp - L), (0, 0), (0, 0))).reshape(n, nb, DIL_BLOCK, h, dh)
    key_idx = np.arange(nb)[:, None] * DIL_BLOCK + np.arange(kw)[None, :]
    kpad = ((0, 0), (half, lp - L + half), (0, 0), (0, 0))
    kb = jnp.take(jnp.pad(kr, kpad), key_idx, axis=1)
    vb = jnp.take(jnp.pad(vr, kpad), key_idx, axis=1)
    qpos = np.arange(nb)[:, None] * DIL_BLOCK + np.arange(DIL_BLOCK)[None, :]
    kpos = key_idx - half
    dist = np.abs(qpos[:, :, None] - kpos[:, None, :])
    valid = (dist <= half) & (kpos[:, None, :] >= 0) & (kpos[:, None, :] < L)
    bias = -slopes[None, :, None, None] * (dist * dilation).astype(np.float32)[:, None]
    sc = jnp.einsum('nbqhd,nbkhd->nbhqk', qb, kb, preferred_element_type=jnp.float32) * (dh ** -0.5)
    sc = jnp.where(valid[:, None], sc + bias, -jnp.inf)
    m = jnp.max(sc, axis=-1, keepdims=True)
    e = jnp.exp(sc - m)
    den = jnp.sum(e, axis=-1, keepdims=True)
    o = jnp.einsum('nbhqk,nbkhd->nbqhd', (e / den).astype(v.dtype), vb).reshape(n, lp, h, dh)[:, :L]
    lse = jnp.swapaxes((m + jnp.log(den))[..., 0], 2, 3).reshape(n, lp, h)[:, :L]

    def from_res(t):
        rest = t.shape[2:]
        return jnp.swapaxes(t.reshape((b, dilation, L) + rest), 1, 2).reshape((b, s) + rest)

    return from_res(o), from_res(lse)


def dilated_attention(q, k, v, slopes):
    outs, lses = [], []
    for window, dilation in DIL_PATTERNS:
        o, l = dilated_branch(q, k, v, slopes, window, dilation)
        outs.append(o)
        lses.append(l)
    w = jax.nn.softmax(jnp.stack(lses), axis=0)
    o = jnp.sum(w[..., None] * jnp.stack(outs).astype(jnp.float32), axis=0)
    return o.astype(q.dtype)


def token_mixer(h, w_in, rpb, gain_na, gain_dil, w_out, slopes):
    b, s, _ = h.shape
    qkv = jnp.einsum('bsd,de->bse', h, w_in).reshape(b, s, 3, N_HEADS, HEAD_DIM)
    q, k, v = qkv[:, :, 0], qkv[:, :, 1], qkv[:, :, 2]
    a = neighborhood_attention(q[:, :, :N_HEADS_NA], k[:, :, :N_HEADS_NA], v[:, :, :N_HEADS_NA], rpb)
    d = dilated_attention(q[:, :, N_HEADS_NA:], k[:, :, N_HEADS_NA:], v[:, :, N_HEADS_NA:], slopes)
    a = rmsnorm(a, gain_na.reshape(N_HEADS_NA, HEAD_DIM))
    d = rmsnorm(d, gain_dil.reshape(N_HEADS_DIL, HEAD_DIM))
    o = jnp.concatenate([a, d], axis=2).reshape(b, s, D_MODEL)
    return jnp.einsum('bsd,de->bse', o, w_out)


def expert_choice_ffn(h, w_router, w_gate, w_up, w_down):
    b, s, d = h.shape
    n = b * s
    cap = CAPACITY_FACTOR * n // N_EXPERTS
    x = h.reshape(n, d)
    aff = jax.nn.softmax(jnp.einsum('nd,de->ne', x, w_router).astype(jnp.float32), axis=-1)
    g, idx = lax.top_k(aff.T, cap)
    xe = x[idx]
    hid = jax.nn.silu(jnp.einsum('ecd,edf->ecf', xe, w_gate)) * jnp.einsum('ecd,edf->ecf', xe, w_up)
    ye = jnp.einsum('ecf,efd->ecd', hid, w_down) * g[..., None].astype(x.dtype)
    y = jnp.zeros_like(x).at[idx.reshape(-1)].add(ye.reshape(-1, d))
    return y.reshape(b, s, d)


def trunk(x, norm1, w_in, rpb, gain_na, gain_dil, w_out, norm2, w_router, w_gate, w_up, w_down, final_norm):
    slopes = alibi_slopes(N_HEADS_DIL)
    for l in range(DEPTH):
        x = x + token_mixer(rmsnorm(x, norm1[l]), w_in[l], rpb[l], gain_na[l], gain_dil[l], w_out[l], slopes)
        x = x + expert_choice_ffn(rmsnorm(x, norm2[l]), w_router[l], w_gate[l], w_up[l], w_down[l])
    return rmsnorm(x, final_norm)


def setup_inputs(seed: int = 0) -> dict:
    key = jax.random.key(seed)
    ks = jax.random.split(key, 14)
    f32 = jnp.float32
    nrm = jax.random.normal
    return {
        'x_prompt': nrm(ks[0], (BATCH, SEQ, D_MODEL), f32),
        'x_sample': nrm(ks[1], (DEC_BATCH, DEC_SEQ, D_MODEL), f32),
        'norm1': 1.0 + 0.01 * nrm(ks[2], (DEPTH, D_MODEL), f32),
        'w_in': nrm(ks[3], (DEPTH, D_MODEL, 3 * D_MODEL), f32) * D_MODEL ** -0.5,
        'rpb': 0.1 * nrm(ks[4], (DEPTH, N_HEADS_NA, 2 * NA_ROWS - 1, 2 * NA_COLS - 1), f32),
        'gain_na': 1.0 + 0.01 * nrm(ks[5], (DEPTH, D_NA), f32),
        'gain_dil': 1.0 + 0.01 * nrm(ks[6], (DEPTH, D_DIL), f32),
        'w_out': nrm(ks[7], (DEPTH, D_MODEL, D_MODEL), f32) * D_MODEL ** -0.5,
        'norm2': 1.0 + 0.01 * nrm(ks[8], (DEPTH, D_MODEL), f32),
        'w_router': nrm(ks[9], (DEPTH, D_MODEL, N_EXPERTS), f32) * D_MODEL ** -0.5,
        'w_gate': nrm(ks[10], (DEPTH, N_EXPERTS, D_MODEL, EXPERT_FF), f32) * D_MODEL ** -0.5,
        'w_up': nrm(ks[11], (DEPTH, N_EXPERTS, D_MODEL, EXPERT_FF), f32) * D_MODEL ** -0.5,
        'w_down': nrm(ks[12], (DEPTH, N_EXPERTS, EXPERT_FF, D_MODEL), f32) * EXPERT_FF ** -0.5,
        'final_norm': 1.0 + 0.01 * nrm(ks[13], (D_MODEL,), f32),
    }


def reference(x_prompt, x_sample, norm1, w_in, rpb, gain_na, gain_dil, w_out, norm2, w_router, w_gate, w_up, w_down, final_norm):
    y_prompt = trunk(x_prompt, norm1, w_in, rpb, gain_na, gain_dil, w_out, norm2, w_router, w_gate, w_up, w_down, final_norm)
    y_sample = trunk(x_sample, norm1, w_in, rpb, gain_na, gain_dil, w_out, norm2, w_router, w_gate, w_up, w_down, final_norm)
    return (y_prompt, y_sample)
```

```python
import numpy as np
from contextlib import ExitStack

import concourse.bass as bass
import concourse.mybir as mybir
from concourse.bass_utils import run_bass_kernel_spmd

F32 = mybir.dt.float32
BF16 = mybir.dt.bfloat16
I32 = mybir.dt.int32
AF = mybir.ActivationFunctionType
ALU = mybir.AluOpType

D_MODEL = 1024
NCH = 8
HEAD_DIM = 64
N_HEADS = 16
N_HEADS_NA = 4
N_EXPERTS = 16
GRID_W = 64
RMS_EPS = 1e-6
SEQ_P = 2048
SEQ_S = 4096
DIL_PATTERNS = ((128, 1), (512, 4), (2048, 16))
N_CORES = 8


class Buf:
    __slots__ = ("name", "last_w", "readers")

    def __init__(self, name):
        self.name = name
        self.last_w = []
        self.readers = []


class _Op:
    __slots__ = ("eng", "fn", "deps", "is_dma", "sig", "sem", "val", "idx", "prewait", "is_cc")

    def __init__(self, eng, fn, is_dma):
        self.eng = eng
        self.fn = fn
        self.deps = []
        self.is_dma = is_dma
        self.is_cc = False
        self.sig = False
        self.sem = None
        self.val = 0
        self.idx = -1
        self.prewait = None


ENGS = ("pe", "act", "dve", "pool", "sp")


class Rec:
    def __init__(self, nc, n_dma_sems=(("sp", 40), ("pool", 6), ("act", 2))):
        self.nc = nc
        self.eng_sem = {e: nc.alloc_semaphore(f"sem_{e}") for e in ("pe", "act", "dve", "pool")}
        self.eng_cnt = {e: 0 for e in self.eng_sem}
        self.dma_sems = {q: [nc.alloc_semaphore(f"dsem_{q}_{i}") for i in range(n)] for q, n in n_dma_sems}
        self.dma_val = {q: [0] * len(self.dma_sems[q]) for q in self.dma_sems}
        self.dma_rr = {q: 0 for q in self.dma_sems}
        self.seen = {e: {} for e in ENGS}
        self.ops = []
        self.cc_sem = nc.alloc_semaphore("sem_cc")
        self.cc_cnt = 0

    def op(self, eng, fn, reads=(), writes=(), dma=False):
        o = _Op(eng, fn, dma)
        deps = []
        for b in reads:
            deps.extend(b.last_w)
        for b in writes:
            deps.extend(b.readers)
            deps.extend(b.last_w)
        o.deps = deps
        for b in reads:
            b.readers.append(o)
        for b in writes:
            b.last_w = [o]
            b.readers = []
        self.ops.append(o)
        return o

    def cc(self, fn, reads=(), writes=()):
        o = self.op("pool", fn, reads, writes)
        o.is_cc = True
        self.cc_cnt += 1
        o.sem = self.cc_sem
        o.val = self.cc_cnt
        return o

    def dma(self, q, out, in_, reads=(), writes=()):
        return self.op(q, lambda e: e.dma_start(out=out, in_=in_), reads, writes, dma=True)

    def flush(self, final=False):
        nc = self.nc
        ops = self.ops
        self.ops = []
        per_eng = {e: [] for e in ENGS}
        for o in ops:
            per_eng[o.eng].append(o)
        for o in ops:
            for d in o.deps:
                if d.eng == "pe" and o.eng == "pe" and not d.is_dma and not o.is_dma:
                    continue
                if not d.is_cc:
                    d.sig = True
        for e in ("pe", "act", "dve", "pool"):
            comp = [o for o in per_eng[e] if not o.is_dma and not o.is_cc]
            if comp:
                comp[-1].sig = True
        for e in ENGS:
            for o in per_eng[e]:
                if o.is_cc:
                    continue
                if o.is_dma:
                    q = o.eng
                    i = self.dma_rr[q]
                    self.dma_rr[q] = (i + 1) % len(self.dma_sems[q])
                    prev = self.dma_val[q][i]
                    o.sem = self.dma_sems[q][i]
                    o.prewait = (o.sem, prev) if prev > 0 else None
                    o.val = prev + 16
                    self.dma_val[q][i] = o.val
                elif o.sig:
                    self.eng_cnt[e] += 1
                    o.sem = self.eng_sem[e]
                    o.val = self.eng_cnt[e]
        dma_final = [(self.dma_sems[q][i], v) for q in self.dma_sems for i, v in enumerate(self.dma_val[q]) if v > 0]

        def emit_engine(e, eng):
            seen = self.seen[e]
            for o in per_eng[e]:
                waits = {}
                for d in o.deps:
                    if d.sem is None:
                        continue
                    if (not d.is_dma) and (not d.is_cc) and d.eng == e == "pe":
                        continue
                    k = id(d.sem)
                    if seen.get(k, 0) >= d.val:
                        continue
                    if k not in waits or waits[k][1] < d.val:
                        waits[k] = (d.sem, d.val)
                if o.prewait is not None:
                    k = id(o.prewait[0])
                    if seen.get(k, 0) < o.prewait[1]:
                        if k not in waits or waits[k][1] < o.prewait[1]:
                            waits[k] = o.prewait
                for k, (s, v) in waits.items():
                    eng.wait_ge(s, v)
                    seen[k] = v
                ins = o.fn(eng)
                if o.is_cc:
                    ins.then_inc(o.sem, 1)
                elif o.is_dma:
                    ins.then_inc(o.sem, 16)
                elif o.sig:
                    ins.then_inc(o.sem, 1)
            for (s, v) in dma_final:
                k = id(s)
                if seen.get(k, 0) < v:
                    eng.wait_ge(s, v)
                    seen[k] = v
            for ce, s in self.eng_sem.items():
                v = self.eng_cnt[ce]
                k = id(s)
                if v > 0 and seen.get(k, 0) < v:
                    eng.wait_ge(s, v)
                    seen[k] = v

        with nc.Block() as block:
            @block.tensor
            def _(eng):
                emit_engine("pe", eng)

            @block.scalar
            def _(eng):
                emit_engine("act", eng)

            @block.vector
            def _(eng):
                emit_engine("dve", eng)

            @block.gpsimd
            def _(eng):
                emit_engine("pool", eng)

            @block.sync
            def _(eng):
                emit_engine("sp", eng)


def alibi_slopes(n):
    import math

    def pow2(m):
        start = 2.0 ** (-8.0 / m)
        return [start ** (i + 1) for i in range(m)]
    if math.log2(n).is_integer():
        s = pow2(n)
    else:
        c = 2 ** int(math.floor(math.log2(n)))
        s = pow2(c) + pow2(2 * c)[0::2][: n - c]
    return np.asarray(s, dtype=np.float32)


NA_CLASSES = ((0, 0), (-2, -2), (-4, -3), (-4, -4), (-6, -6))


def na_start(r, rows):
    return min(max(r - 4, 0), rows - 8)


def na_class(i, rows):
    c = (na_start(2 * i, rows) - 2 * i, na_start(2 * i + 1, rows) - 2 * i)
    return NA_CLASSES.index(c)


def na_bias_index():
    ncls = len(NA_CLASSES)
    ri = np.full((ncls, 128, 5, 128), -1, np.int64)
    ci = np.full((ncls, 128, 5, 128), -1, np.int64)
    for c, (c0, c1) in enumerate(NA_CLASSES):
        cq = (c0, c1)
        for t in range(5):
            for m in range(128):
                krr = (t * 128 + m) // 64
                kc = m % 64
                for u in range(128):
                    qr = u // 64
                    qc = u % 64
                    rel = krr + c0 - cq[qr]
                    if rel < 0 or rel >= 8:
                        continue
                    win0 = min(max(qc - 8, 0), GRID_W - 16)
                    if kc < win0 or kc >= win0 + 16:
                        continue
                    ro = (c0 + krr) - qr
                    co = kc - qc
                    ri[c, m, t, u] = ro + 7
                    ci[c, m, t, u] = co + 15
    return ri, ci


def dil_mask_const():
    slopes = alibi_slopes(N_HEADS - N_HEADS_NA).astype(np.float64)
    m = np.arange(128)[:, None]
    u = np.arange(128)[None, :]
    out = np.zeros((128, 36, 3, 128), np.float32)
    for h in range(12):
        for pi, (_, D) in enumerate(DIL_PATTERNS):
            dA = np.abs(m - 64 - u)
            dB = np.abs(m + 64 - u)
            out[:, h * 3 + pi, 0, :] = np.where(dA <= 64, np.exp(-slopes[h] * dA * D), 0.0)
            out[:, h * 3 + pi, 1, :] = np.where(dB <= 64, np.exp(-slopes[h] * dB * D), 0.0)
            out[0:64, h * 3 + pi, 2, :] = out[64:128, h * 3 + pi, 0, :]
    return out


class Cfg:
    def __init__(self, n_prompt=4, depth=4, with_sample=True):
        self.NP = n_prompt
        self.depth = depth
        self.seqs = [(i * SEQ_P, SEQ_P) for i in range(n_prompt)]
        self.TP = n_prompt * SEQ_P
        if with_sample:
            self.seqs.append((self.TP, SEQ_S))
        self.TS = SEQ_S if with_sample else 0
        self.T = self.TP + self.TS
        self.NB = self.T // 512


class K:
    pass


_UID = [0]


def _sb(es, nc, name, shape, dt):
    _UID[0] += 1
    return es.enter_context(nc.sbuf_tensor(f"{name}_u{_UID[0]}", list(shape), dt))


def _ps(es, nc, name):
    _UID[0] += 1
    return es.enter_context(nc.psum_tensor(f"{name}_u{_UID[0]}", [128, 512], F32))


def build_program(cfg, stop_after=None, debug=False, direct_w=False):
    nc = bass.Bass("TRN2", target_bir_lowering=False)
    k = K()
    k.nc, k.cfg = nc, cfg
    L, T = cfg.depth, cfg.T
    ncls = len(NA_CLASSES)
    din = lambda name, shape, dt=F32: nc.dram_tensor(name, list(shape), dt, kind="ExternalInput").ap()
    k.xin = din("xin", [T, D_MODEL])
    k.norm1g = din("norm1g", [L, 128, NCH])
    k.w_in_s = din("w_in_s", [L * 128, 3 * D_MODEL])
    nab_rows = L * N_HEADS_NA * ncls * 128
    k.nabias_s = din("nabias_s", [nab_rows // N_CORES, 5 * 128])
    k.dilmask = din("dilmask", [128, 36 * 3 * 128], BF16)
    k.gattn = din("gattn", [L, 128, NCH])
    k.w_out_s = din("w_out_s", [L * 128, D_MODEL])
    k.norm2g = din("norm2g", [L, 128, NCH])
    k.w_router = din("w_router", [L, D_MODEL, N_EXPERTS])
    k.w_gate_s = din("w_gate_s", [L * 2 * D_MODEL, D_MODEL])
    k.w_up_s = din("w_up_s", [L * 2 * D_MODEL, D_MODEL])
    k.w_down_s = din("w_down_s", [L * 2 * D_MODEL, D_MODEL])
    k.finalg = din("finalg", [128, NCH])
    k.gmat = din("gmat", [128, 2, 128])
    k.yout = nc.dram_tensor("yout", [T, D_MODEL], F32, kind="ExternalOutput").ap()
    dbgset = set(debug) if debug else set()
    scr = lambda name, shape, dt: nc.dram_tensor(name, list(shape), dt, kind=("ExternalOutput" if name in dbgset else "Internal")).ap()
    k.xT = scr("xT", [D_MODEL, T], F32)
    k.qT = scr("qT", [D_MODEL, T], BF16)
    k.kT = scr("kT", [D_MODEL, T], BF16)
    k.vA = scr("vA", [T, N_HEADS, 128], BF16)
    k.oT = scr("oT", [D_MODEL, T], BF16)
    k.h2T = scr("h2T", [D_MODEL, T], BF16)
    k.gmT = scr("gmT", [N_EXPERTS, T], F32)
    k.thr_dbg = scr("thr_dbg", [128, 4], F32) if "thr_dbg" in dbgset else None
    k.ag_in = nc.dram_tensor("ag_in", [N_EXPERTS, T], F32).ap()
    k.ag_out = nc.dram_tensor("ag_out", [N_CORES * N_EXPERTS, T], F32).ap()
    k.aff_dbg = scr("aff_dbg", [N_EXPERTS, T], F32) if "aff_dbg" in dbgset else None
    rec = Rec(nc)
    k.rec = rec
    itn = lambda name, shape, dt=F32: nc.dram_tensor(name, list(shape), dt, kind=("ExternalInput" if (direct_w and not name.startswith("ccin_")) else "Internal")).ap()
    k.w_in = [itn(f"w_in_f{l}", [D_MODEL, 3 * D_MODEL]) for l in range(L)]
    k.w_out = [itn(f"w_out_f{l}", [D_MODEL, D_MODEL]) for l in range(L)]
    k.w_gate = [itn(f"w_gate_f{l}", [N_EXPERTS * D_MODEL, D_MODEL]) for l in range(L)]
    k.w_up = [itn(f"w_up_f{l}", [N_EXPERTS * D_MODEL, D_MODEL]) for l in range(L)]
    k.w_down = [itn(f"w_down_f{l}", [N_EXPERTS * D_MODEL, D_MODEL]) for l in range(L)]
    k.nabias = itn("nabias_f", [nab_rows, 5 * 128])
    k.wbuf = {}
    groups = [list(range(N_CORES))]

    def gather(name, src, dst):
        if direct_w:
            k.wbuf[name] = Buf("direct_" + name)
            return
        tmp = itn("ccin_" + name, list(src.shape))
        tb, fb = Buf("ccin_" + name), Buf("ccout_" + name)
        nrow = src.shape[0]
        step = max(1, min(nrow, (1 << 20) // (src.shape[1] * 4)))
        for r0 in range(0, nrow, step):
            r1 = min(nrow, r0 + step)
            o = rec.dma("sp", tmp[r0:r1, :], src[r0:r1, :])
            tb.last_w.append(o)
        rec.cc(lambda e: e.collective_compute("AllGather", ALU.bypass, replica_groups=groups, ins=[tmp], outs=[dst]),
               reads=[tb], writes=[fb])
        k.wbuf[name] = fb

    gather("nabias", k.nabias_s, k.nabias)
    for l in range(L):
        gather(f"w_in{l}", k.w_in_s[l * 128:(l + 1) * 128, :], k.w_in[l])
        gather(f"w_out{l}", k.w_out_s[l * 128:(l + 1) * 128, :], k.w_out[l])
        for nm, src, dst in (("w_gate", k.w_gate_s, k.w_gate), ("w_up", k.w_up_s, k.w_up), ("w_down", k.w_down_s, k.w_down)):
            gather(f"{nm}{l}", src[l * 2 * D_MODEL:(l + 1) * 2 * D_MODEL, :], dst[l])

    with ExitStack() as top:
        k.identF = _sb(top, nc, "identF", [128, 128], F32)
        k.ones_rms = _sb(top, nc, "ones_rms", [128, 128], BF16)
        k.wred = _sb(top, nc, "wred", [128, 2, 64], BF16)
        k.onesF16 = _sb(top, nc, "onesF16", [16, 16], F32)
        k.selF = _sb(top, nc, "selF", [16, N_EXPERTS, 128], F32)
        cB = Buf("consts")
        rec.op("pool", lambda e: e.memset(k.identF[:], 0.0), writes=[cB])
        rec.op("pool", lambda e: e.affine_select(out=k.identF[:], in_=k.identF[:], pattern=[[-1, 128]],
                                                 compare_op=ALU.not_equal, fill=1.0, base=0, channel_multiplier=1),
               writes=[cB])
        rec.op("pool", lambda e: e.memset(k.ones_rms[:], 1.0 / 1024.0), writes=[cB])
        rec.op("pool", lambda e: e.memset(k.wred[0:64, 0, :], 1.0 / 64.0), writes=[cB])
        rec.op("pool", lambda e: e.memset(k.wred[64:128, 0, :], RMS_EPS / 64.0), writes=[cB])
        rec.op("pool", lambda e: e.memset(k.wred[0:64, 1, :], RMS_EPS / 64.0), writes=[cB])
        rec.op("pool", lambda e: e.memset(k.wred[64:128, 1, :], 1.0 / 64.0), writes=[cB])
        rec.op("pool", lambda e: e.memset(k.onesF16[:], 1.0), writes=[cB])
        rec.op("pool", lambda e: e.memset(k.selF[:], 0.0), writes=[cB])
        rec.op("pool", lambda e: e.affine_select(out=k.selF[:], in_=k.selF[:], pattern=[[-1, N_EXPERTS], [0, 128]],
                                                 compare_op=ALU.not_equal, fill=1.0, base=0, channel_multiplier=1),
               writes=[cB])
        rec.flush()

        phase_P0(k)
        if stop_after == "P0":
            return nc
        for l in range(L):
            phase_A1(k, l)
            if stop_after == f"A1.{l}":
                return nc
            phase_A2(k, l)
            if stop_after == f"A2.{l}":
                return nc
            phase_A3(k, l)
            if stop_after == f"A3.{l}":
                return nc
            phase_R(k, l)
            if stop_after == f"R.{l}":
                return nc
            phase_F(k, l)
            if stop_after == f"F.{l}":
                return nc
        phase_Z(k)
    return nc


def _evac(rec, i, out, in_, reads, writes, scale=None):
    if i % 2 == 0:
        if scale is None:
            rec.op("act", lambda e: e.activation(out=out, in_=in_, func=AF.Copy), reads, writes)
        else:
            rec.op("act", lambda e: e.activation(out=out, in_=in_, func=AF.Copy, scale=float(scale)), reads, writes)
    else:
        if scale is None:
            rec.op("dve", lambda e: e.tensor_copy(out=out, in_=in_), reads, writes)
        else:
            rec.op("dve", lambda e: e.tensor_scalar(out=out, in0=in_, scalar1=float(scale), scalar2=None, op0=ALU.mult),
                   reads, writes)


def phase_P0(k):
    nc, rec, cfg = k.nc, k.rec, k.cfg
    with ExitStack() as es:
        xt = [_sb(es, nc, f"p0_x{i}", [128, D_MODEL], F32) for i in range(8)]
        xtB = [Buf(f"p0_x{i}") for i in range(8)]
        st = [_sb(es, nc, f"p0_st{i}", [128, NCH, 512], F32) for i in range(2)]
        stB = [Buf(f"p0_st{i}") for i in range(2)]
        ps = [_ps(es, nc, f"p0_ps{i}") for i in range(4)]
        psB = [Buf(f"p0_ps{i}") for i in range(4)]
        xTv = k.xT.rearrange("(c p) t -> p c t", p=128)
        n = 0
        for b in range(cfg.NB):
            for s in range(4):
                i = (b % 2) * 4 + s
                r0 = (b * 4 + s) * 128
                rec.dma("sp", xt[i][:], k.xin[r0:r0 + 128, :], writes=[xtB[i]])
            sti = b % 2
            for c in range(NCH):
                pi = n % 4
                n += 1
                for s in range(4):
                    i = (b % 2) * 4 + s
                    rec.op("pe", lambda e, pi=pi, s=s, i=i, c=c: e.transpose(ps[pi][:, s * 128:(s + 1) * 128],
                                                                          xt[i][:, c * 128:(c + 1) * 128], k.identF[:]),
                           reads=[xtB[i]], writes=[psB[pi]])
                _evac(rec, c, st[sti][:, c, :], ps[pi][:, :], [psB[pi]], [stB[sti]])
            rec.dma("sp", xTv[:, :, b * 512:(b + 1) * 512], st[sti][:], reads=[stB[sti]])
        rec.flush()


def _rms_block(k, rec, xb, xbB, sqb, sqB, ps_ss, ssB, tmp, tmpB, rstd, rstdB):
    rec.op("act", lambda e: e.activation(out=sqb[:], in_=xb[:], func=AF.Square), reads=[xbB], writes=[sqB])
    for c in range(NCH):
        rec.op("pe", lambda e, c=c: e.matmul(ps_ss[:, :], k.ones_rms[:], sqb[:, c, :], start=(c == 0), stop=(c == NCH - 1)),
               reads=[sqB], writes=[ssB])
    rec.op("act", lambda e: e.activation(out=tmp[:], in_=ps_ss[:, :], func=AF.Sqrt, bias=k.epsT[:, 0:1], scale=1.0),
           reads=[ssB], writes=[tmpB])
    rec.op("dve", lambda e: e.reciprocal(out=rstd[:], in_=tmp[:]), reads=[tmpB], writes=[rstdB])


def phase_A1(k, l):
    nc, rec, cfg = k.nc, k.rec, k.cfg
    with ExitStack() as es:
        w = _sb(es, nc, "a1_w", [128, NCH, 3 * D_MODEL], BF16)
        wB = [Buf(f"a1_w{c}") for c in range(NCH)]
        g1 = _sb(es, nc, "a1_g", [128, NCH], F32)
        gB = Buf("a1_g")
        k.epsT = _sb(es, nc, "a1_eps", [128, 1], F32)
        rec.op("dve", lambda e: e.memset(k.epsT[:], RMS_EPS), writes=[gB])
        rec.dma("sp", g1[:], k.norm1g[l], writes=[gB])
        for c in range(NCH):
            rec.dma("pool", w[:, c, :], k.w_in[l][c * 128:(c + 1) * 128, :], reads=[k.wbuf[f"w_in{l}"]], writes=[wB[c]])
        xb = [_sb(es, nc, f"a1_x{i}", [128, NCH, 512], F32) for i in range(2)]
        xbB = [Buf(f"a1_x{i}") for i in range(2)]
        sqb = _sb(es, nc, "a1_sq", [128, NCH, 512], BF16)
        sqB = Buf("a1_sq")
        tmp = _sb(es, nc, "a1_tmp", [128, 512], F32)
        tmpB = Buf("a1_tmp")
        rstd = _sb(es, nc, "a1_rstd", [128, 512], F32)
        rstdB = Buf("a1_rstd")
        hT = [_sb(es, nc, f"a1_h{i}", [128, NCH, 512], BF16) for i in range(2)]
        hB = [Buf(f"a1_h{i}") for i in range(2)]
        stq = [_sb(es, nc, f"a1_stq{i}", [128, 16, 512], BF16) for i in range(2)]
        stqB = [Buf(f"a1_stq{i}") for i in range(2)]
        stv = [_sb(es, nc, f"a1_stv{i}", [128, 4, N_HEADS, 128], BF16) for i in range(2)]
        stvB = [Buf(f"a1_stv{i}") for i in range(2)]
        ps_ss = _ps(es, nc, "a1_ss")
        ssB = Buf("a1_ss")
        ps = [_ps(es, nc, f"a1_ps{i}") for i in range(5)]
        psB = [Buf(f"a1_ps{i}") for i in range(5)]
        for i in range(2):
            rec.op("pool", lambda e, i=i: e.memset(stv[i][:], 1.0), writes=[stvB[i]])
        xTv = k.xT.rearrange("(c p) t -> p c t", p=128)
        qTv = k.qT.rearrange("(c p) t -> p c t", p=128)
        kTv = k.kT.rearrange("(c p) t -> p c t", p=128)
        vAv = k.vA.rearrange("(n p) h c -> p n h c", p=128)
        n = 0
        for b in range(cfg.NB):
            bi = b % 2
            blk = slice(b * 512, (b + 1) * 512)
            rec.dma("sp", xb[bi][:], xTv[:, :, blk], writes=[xbB[bi]])
            _rms_block(k, rec, xb[bi], xbB[bi], sqb, sqB, ps_ss, ssB, tmp, tmpB, rstd, rstdB)
            for c in range(NCH):
                rec.op("dve", lambda e, c=c, bi=bi: e.scalar_tensor_tensor(out=hT[bi][:, c, :], in0=xb[bi][:, c, :],
                                                                           scalar=g1[:, c:c + 1], in1=rstd[:],
                                                                           op0=ALU.mult, op1=ALU.mult),
                       reads=[xbB[bi], rstdB, gB], writes=[hB[bi]])
            for oc in range(16):
                pi = n % 5
                n += 1
                for kc in range(NCH):
                    rec.op("pe", lambda e, pi=pi, kc=kc, oc=oc, bi=bi: e.matmul(ps[pi][:, :], w[:, kc, oc * 128:(oc + 1) * 128],
                                                                               hT[bi][:, kc, :], start=(kc == 0), stop=(kc == NCH - 1)),
                           reads=[wB[kc], hB[bi]], writes=[psB[pi]])
                _evac(rec, oc, stq[bi][:, oc, :], ps[pi][:, :], [psB[pi]], [stqB[bi]], scale=(0.125 if oc < 8 else None))
            rec.dma("sp", qTv[:, :, blk], stq[bi][:, 0:8, :], reads=[stqB[bi]])
            rec.dma("sp", kTv[:, :, blk], stq[bi][:, 8:16, :], reads=[stqB[bi]])
            for s in range(4):
                for hf in range(2):
                    pi = n % 5
                    n += 1
                    for kc in range(NCH):
                        rec.op("pe", lambda e, pi=pi, kc=kc, s=s, hf=hf, bi=bi: e.matmul(
                            ps[pi][:, :], hT[bi][:, kc, s * 128:(s + 1) * 128],
                            w[:, kc, 2 * D_MODEL + hf * 512: 2 * D_MODEL + (hf + 1) * 512],
                            start=(kc == 0), stop=(kc == NCH - 1)), reads=[wB[kc], hB[bi]], writes=[psB[pi]])
                    pv = ps[pi][:, :].rearrange("p (hp par d) -> p hp par d", par=2, d=64)
                    sv = stv[bi][:, s, hf * 8:(hf + 1) * 8, :].rearrange("p (hp par) c -> p hp par c", par=2)
                    _evac(rec, 0, sv[:, :, 0, 0:64], pv[:, :, 0, :], [psB[pi]], [stvB[bi]])
                    _evac(rec, 1, sv[:, :, 1, 64:128], pv[:, :, 1, :], [psB[pi]], [stvB[bi]])
            rec.dma("sp", vAv[:, b * 4:(b + 1) * 4, :, :], stv[bi][:], reads=[stvB[bi]])
        rec.flush()


def phase_A2(k, l):
    nc, rec, cfg = k.nc, k.rec, k.cfg
    ncls = len(NA_CLASSES)
    with ExitStack() as es:
        dms = [_sb(es, nc, f"a2_dm{i}", [128, 6, 3, 128], BF16) for i in range(2)]
        dmBs = [Buf(f"a2_dm{i}") for i in range(2)]
        qd = _sb(es, nc, "a2_qd", [128, 4096 if cfg.TS else 2048], BF16)
        kd = _sb(es, nc, "a2_kd", [128, 4096 if cfg.TS else 2048], BF16)
        qdB, kdB = Buf("a2_qd"), Buf("a2_kd")
        import os as _os2
        if _os2.environ.get("A2_PAD"):
            _pad = _sb(es, nc, "a2_pad", [128, int(_os2.environ["A2_PAD"])], BF16)
        nab = _sb(es, nc, "a2_nab", [128, N_HEADS_NA, ncls, 5 * 128], BF16)
        nabB = Buf("a2_nab")
        nraw = [_sb(es, nc, f"a2_nraw{i}", [128, 5 * 128], F32) for i in range(2)]
        nrawB = [Buf(f"a2_nraw{i}") for i in range(2)]
        nabv = k.nabias.rearrange("(s p) c -> s p c", p=128)
        for h in range(N_HEADS_NA):
            for c in range(ncls):
                i = (h * ncls + c) % 2
                rec.dma("sp", nraw[i][:], nabv[(l * N_HEADS_NA + h) * ncls + c], reads=[k.wbuf["nabias"]], writes=[nrawB[i]])
                rec.op("act", lambda e, i=i, h=h, c=c: e.activation(out=nab[:, h, c, :], in_=nraw[i][:], func=AF.Exp),
                       reads=[nrawB[i]], writes=[nabB])
        ga = _sb(es, nc, "a2_ga", [128, NCH], F32)
        gaB = Buf("a2_ga")
        rec.dma("sp", ga[:], k.gattn[l], writes=[gaB])
        SM = SEQ_S if cfg.TS else SEQ_P
        qt = _sb(es, nc, "a2_q", [128, SM], BF16)
        kt = _sb(es, nc, "a2_k", [128, SM], BF16)
        qB, kB = Buf("a2_q"), Buf("a2_k")
        NVT = 16 * (SM // 2048 + 1)
        va = [_sb(es, nc, f"a2_va{i}", [128, NVT, 2, 128], BF16) for i in range(2)]
        vaB = [Buf(f"a2_va{i}") for i in range(2)]
        acc = [_sb(es, nc, f"a2_acc{i}", [128, SM], F32) for i in range(2)]
        accB = [Buf(f"a2_acc{i}") for i in range(2)]
        ot = [_sb(es, nc, "a2_ot0", [128, SM], BF16)] * 2
        otB = [Buf("a2_ot0")] * 2
        NE = 3
        E = [_sb(es, nc, f"a2_E{i}", [128, 5 * 128], BF16) for i in range(NE)]
        EB = [Buf(f"a2_E{i}") for i in range(NE)]
        Em = [_sb(es, nc, f"a2_Em{i}", [128, 5 * 128], BF16) for i in range(NE)]
        EmB = [Buf(f"a2_Em{i}") for i in range(NE)]
        sqh = [_sb(es, nc, f"a2_sq{i}", [128, 512], BF16) for i in range(2)]
        sqhB = [Buf(f"a2_sq{i}") for i in range(2)]
        t1 = [_sb(es, nc, f"a2_t1{i}", [128, 512], F32) for i in range(2)]
        t1B = [Buf(f"a2_t1{i}") for i in range(2)]
        rinv = [_sb(es, nc, f"a2_ri{i}", [128, 512], F32) for i in range(2)]
        rinvB = [Buf(f"a2_ri{i}") for i in range(2)]
        psS = [_ps(es, nc, f"a2_psS{i}") for i in range(NE)]
        psSB = [Buf(f"a2_psS{i}") for i in range(NE)]
        psH = [_ps(es, nc, f"a2_psH{i}") for i in range(2)]
        psHB = [Buf(f"a2_psH{i}") for i in range(2)]
        psO = [_ps(es, nc, f"a2_psO{i}") for i in range(2)]
        psOB = [Buf(f"a2_psO{i}") for i in range(2)]
        psR = _ps(es, nc, "a2_psR")
        psRB = Buf("a2_psR")
        cnt = {"e": 0, "o": 0, "h": 0, "f": 0, "v": 0}
        import os as _os3
        SERIAL = bool(_os3.environ.get("A2_SERIAL"))
        LAG = 0 if SERIAL else 2

        for (s0, S) in cfg.seqs:
            R = S // GRID_W
            nqt_all = S // 128
            vseq = k.vA[s0:s0 + S]
            import os as _os
            _only = _os.environ.get("A2_ONLY", "")
            for j in range(NCH):
                if _only and str(j) not in _only.split(":")[0].split(","):
                    continue
                rec.dma("sp", qt[:, 0:S], k.qT[j * 128:(j + 1) * 128, s0:s0 + S], writes=[qB])
                rec.dma("sp", kt[:, 0:S], k.kT[j * 128:(j + 1) * 128, s0:s0 + S], writes=[kB])
                oti = cnt["o"] % 2
                cnt["o"] += 1
                if j < N_HEADS_NA // 2:
                    vi = cnt["v"] % 2
                    cnt["v"] += 1
                    vsrc = vseq.rearrange("(n p) h c -> p n h c", p=128)[:, :, 2 * j:2 * j + 2, :]
                    for n0 in range(0, nqt_all, 4):
                        o = rec.dma("sp", va[vi][:, n0:n0 + 4, :, :], vsrc[:, n0:n0 + 4, :, :], writes=[vaB[vi]] if n0 == 0 else [])
                        if n0:
                            vaB[vi].last_w.append(o)
                    for hh in range(2):
                        h = 2 * j + hh
                        rows = slice(hh * 64, hh * 64 + 64)
                        pend = []

                        def na_scores(i, hh=hh, h=h, rows=rows, vi=vi):
                            ei = cnt["e"] % NE
                            cnt["e"] += 1
                            rs = na_start(2 * i, R)
                            nrows = na_start(2 * i + 1, R) + 8 - rs
                            half = nrows == 9
                            kt0 = rs // 2
                            cls = na_class(i, R)
                            qv = qt[rows, i * 128:(i + 1) * 128]
                            for t in range(4):
                                rec.op("pe", lambda e, t=t: e.matmul(psS[ei][:, t * 128:(t + 1) * 128],
                                                                    kt[rows, (kt0 + t) * 128:(kt0 + t + 1) * 128], qv,
                                                                    start=True, stop=True),
                                       reads=[qB, kB], writes=[psSB[ei]])
                            rec.op("act", lambda e: e.activation(out=E[ei][:, 0:512], in_=psS[ei][:, :], func=AF.Exp),
                                   reads=[psSB[ei]], writes=[EB[ei]])
                            rec.op("dve", lambda e: e.tensor_tensor(out=Em[ei][:, 0:512], in0=E[ei][:, 0:512],
                                                                    in1=nab[:, h, cls, 0:512], op=ALU.mult),
                                   reads=[EB[ei], nabB], writes=[EmB[ei]])
                            hi = None
                            if half:
                                hi = cnt["h"] % 2
                                cnt["h"] += 1
                                rec.op("pe", lambda e: e.matmul(psH[hi][0:64, 0:128],
                                                                kt[rows, (kt0 + 4) * 128:(kt0 + 4) * 128 + 64], qv,
                                                                start=True, stop=True),
                                       reads=[qB, kB], writes=[psHB[hi]])
                                rec.op("act", lambda e: e.activation(out=E[ei][0:64, 512:640], in_=psH[hi][0:64, 0:128], func=AF.Exp),
                                       reads=[psHB[hi]], writes=[EB[ei]])
                                rec.op("dve", lambda e: e.tensor_tensor(out=Em[ei][0:64, 512:640], in0=E[ei][0:64, 512:640],
                                                                        in1=nab[0:64, h, cls, 512:640], op=ALU.mult),
                                       reads=[EB[ei], nabB], writes=[EmB[ei]])
                            return (i, ei, kt0, half)

                        def na_pv(st, hh=hh, vi=vi):
                            i, ei, kt0, half = st
                            oi = cnt["f"] % 2
                            cnt["f"] += 1
                            for t in range(4):
                                rec.op("pe", lambda e, t=t: e.matmul(psO[oi][:, 0:128], va[vi][:, kt0 + t, hh, :],
                                                                    Em[ei][:, t * 128:(t + 1) * 128],
                                                                    start=(t == 0), stop=(t == 3 and not half)),
                                       reads=[vaB[vi], EmB[ei]], writes=[psOB[oi]])
                            if half:
                                rec.op("pe", lambda e: e.matmul(psO[oi][:, 0:128], va[vi][0:64, kt0 + 4, hh, :],
                                                                Em[ei][0:64, 512:640], start=False, stop=True),
                                       reads=[vaB[vi], EmB[ei]], writes=[psOB[oi]])
                            rec.op("act", lambda e: e.activation(out=acc[hh][:, i * 128:(i + 1) * 128], in_=psO[oi][:, 0:128], func=AF.Copy),
                                   reads=[psOB[oi]], writes=[accB[hh]])

                        for i in range(nqt_all + LAG):
                            if i < nqt_all:
                                pend.append(na_scores(i))
                            if i >= LAG:
                                na_pv(pend.pop(0))
                                if SERIAL:
                                    rec.flush()
                else:
                    dmi = j % 2
                    dm, dmB = dms[dmi], dmBs[dmi]
                    h0 = (2 * j - N_HEADS_NA) * 3
                    rec.dma("sp", dm[:].rearrange("p a b c -> p (a b c)"), k.dilmask[:, h0 * 384:(h0 + 6) * 384], writes=[dmB])
                    for pi, (_, D) in enumerate(DIL_PATTERNS):
                        if _only and ":" in _only and str(pi) not in _only.split(":")[1].split(","):
                            continue
                        Lr = S // D
                        nqt = Lr // 128
                        vi = cnt["v"] % 2
                        cnt["v"] += 1
                        vres = vseq.rearrange("(q r) h c -> r q h c", r=D)
                        first = True
                        for r in range(D):
                            base = r * (nqt + 1)
                            o1 = rec.dma("sp", va[vi][0:64, base, :, :], vres[r, 0:64, 2 * j:2 * j + 2, :],
                                         writes=[vaB[vi]] if first else [])
                            if not first:
                                vaB[vi].last_w.append(o1)
                            first = False
                            o2 = rec.dma("sp", va[vi][0:64, base + nqt, :, :], vres[r, Lr - 64:Lr, 2 * j:2 * j + 2, :])
                            vaB[vi].last_w.append(o2)
                            if nqt > 1:
                                src = vres[r, 64:64 + 128 * (nqt - 1), 2 * j:2 * j + 2, :].rearrange("(kk m) h c -> m kk h c", m=128)
                                for n0 in range(0, nqt - 1, 4):
                                    n1 = min(nqt - 1, n0 + 4)
                                    o3 = rec.dma("sp", va[vi][:, base + 1 + n0:base + 1 + n1, :, :], src[:, n0:n1, :, :])
                                    vaB[vi].last_w.append(o3)
                        for hh in range(2):
                            h12 = 2 * j + hh - N_HEADS_NA
                            rows = slice(hh * 64, hh * 64 + 64)
                            mk = dm[:, hh * 3 + pi, :, :]
                            if D == 1:
                                qres = qt[rows, 0:S].rearrange("p (q r) -> p r q", r=D)
                                kres = kt[rows, 0:S].rearrange("p (q r) -> p r q", r=D)
                                qBx, kBx = qB, kB
                            else:
                                rec.op("act", lambda e, rows=rows, D=D: e.activation(out=qd[rows, 0:S].rearrange("p (r q) -> p r q", r=D),
                                                                                   in_=qt[rows, 0:S].rearrange("p (q r) -> p r q", r=D), func=AF.Copy),
                                       reads=[qB], writes=[qdB])
                                rec.op("dve", lambda e, rows=rows, D=D: e.tensor_copy(out=kd[rows, 0:S].rearrange("p (r q) -> p r q", r=D),
                                                                                     in_=kt[rows, 0:S].rearrange("p (q r) -> p r q", r=D)),
                                       reads=[kB], writes=[kdB])
                                qres = qd[rows, 0:S].rearrange("p (r q) -> p r q", r=D)
                                kres = kd[rows, 0:S].rearrange("p (r q) -> p r q", r=D)
                                qBx, kBx = qdB, kdB
                            accv = acc[hh][:, 0:S].rearrange("p (q r) -> p r q", r=D)
                            pend = []

                            def dl_scores(r, i, hh=hh, mk=mk, qres=qres, kres=kres, rows=rows, nqt=nqt, Lr=Lr, qB=qBx, kB=kBx, dmB=dmB):
                                ei = cnt["e"] % NE
                                cnt["e"] += 1
                                qv = qres[:, r, i * 128:(i + 1) * 128]
                                pA = slice(0, 64) if i == 0 else slice(0, 128)
                                pB = slice(0, 64) if i == nqt - 1 else slice(0, 128)
                                kA = kres[:, r, 0:64] if i == 0 else kres[:, r, i * 128 - 64:i * 128 + 64]
                                kBv = kres[:, r, Lr - 64:Lr] if i == nqt - 1 else kres[:, r, i * 128 + 64:i * 128 + 192]
                                rec.op("pe", lambda e: e.matmul(psS[ei][pA, 0:128], kA, qv, start=True, stop=True),
                                       reads=[qB, kB], writes=[psSB[ei]])
                                rec.op("pe", lambda e: e.matmul(psS[ei][pB, 128:256], kBv, qv, start=True, stop=True),
                                       reads=[qB, kB], writes=[psSB[ei]])
                                if i == 0 or i == nqt - 1:
                                    for (pp, cs) in ((pA, slice(0, 128)), (pB, slice(128, 256))):
                                        rec.op("act", lambda e, pp=pp, cs=cs: e.activation(out=E[ei][pp, cs], in_=psS[ei][pp, cs], func=AF.Exp),
                                               reads=[psSB[ei]], writes=[EB[ei]])
                                        ab = (2 if i == 0 else 0) if cs.start == 0 else 1
                                        rec.op("dve", lambda e, pp=pp, cs=cs, ab=ab: e.tensor_tensor(out=Em[ei][pp, cs], in0=E[ei][pp, cs],
                                                                                                    in1=mk[pp, ab, :], op=ALU.mult),
                                               reads=[EB[ei], dmB], writes=[EmB[ei]])
                                else:
                                    rec.op("act", lambda e: e.activation(out=E[ei][:, 0:256], in_=psS[ei][:, 0:256], func=AF.Exp),
                                           reads=[psSB[ei]], writes=[EB[ei]])
                                    rec.op("dve", lambda e: e.tensor_tensor(out=Em[ei][:, 0:256], in0=E[ei][:, 0:256],
                                                                            in1=mk[:, 0:2, :].rearrange("p a b -> p (a b)"), op=ALU.mult),
                                           reads=[EB[ei], dmB], writes=[EmB[ei]])
                                return (r, i, ei, pA, pB)

                            def dl_pv(st, hh=hh, vi=vi, nqt=nqt, accv=accv, pi=pi):
                                r, i, ei, pA, pB = st
                                oi = cnt["f"] % 2
                                cnt["f"] += 1
                                base = r * (nqt + 1)
                                rec.op("pe", lambda e: e.matmul(psO[oi][:, 0:128], va[vi][pA, base + i, hh, :], Em[ei][pA, 0:128],
                                                                start=True, stop=False),
                                       reads=[vaB[vi], EmB[ei]], writes=[psOB[oi]])
                                rec.op("pe", lambda e: e.matmul(psO[oi][:, 0:128], va[vi][pB, base + i + 1, hh, :], Em[ei][pB, 128:256],
                                                                start=False, stop=True),
                                       reads=[vaB[vi], EmB[ei]], writes=[psOB[oi]])
                                av = accv[:, r, i * 128:(i + 1) * 128]
                                if pi == 0:
                                    rec.op("act", lambda e: e.activation(out=av, in_=psO[oi][:, 0:128], func=AF.Copy),
                                           reads=[psOB[oi]], writes=[accB[hh]])
                                else:
                                    rec.op("dve", lambda e: e.tensor_tensor(out=av, in0=av, in1=psO[oi][:, 0:128], op=ALU.add),
                                           reads=[psOB[oi], accB[hh]], writes=[accB[hh]])

                            units = [(r, i) for r in range(D) for i in range(nqt)]
                            for n in range(len(units) + LAG):
                                if n < len(units):
                                    pend.append(dl_scores(*units[n]))
                                if n >= LAG:
                                    dl_pv(pend.pop(0))
                                    if SERIAL:
                                        rec.flush()
                for hh in range(2):
                    rows = slice(hh * 64, hh * 64 + 64)
                    for n0 in range(0, S, 512):
                        ti = cnt["h"] % 2
                        cnt["h"] += 1
                        cs = slice(n0, n0 + 512)
                        rec.op("act", lambda e, ti=ti, hh=hh, cs=cs: e.activation(out=sqh[ti][:], in_=acc[hh][:, cs], func=AF.Square),
                               reads=[accB[hh]], writes=[sqhB[ti]])
                        rec.op("pe", lambda e, ti=ti, hh=hh, rows=rows: e.matmul(psR[rows, :], k.wred[:, hh, :], sqh[ti][:], start=True, stop=True),
                               reads=[sqhB[ti]], writes=[psRB])
                        rec.op("act", lambda e, ti=ti, rows=rows: e.activation(out=t1[ti][rows, :], in_=psR[rows, :], func=AF.Sqrt),
                               reads=[psRB], writes=[t1B[ti]])
                        rec.op("dve", lambda e, ti=ti, rows=rows: e.reciprocal(out=rinv[ti][rows, :], in_=t1[ti][rows, :]),
                               reads=[t1B[ti]], writes=[rinvB[ti]])
                        rec.op("dve", lambda e, ti=ti, rows=rows, hh=hh, cs=cs, oti=oti, j=j: e.scalar_tensor_tensor(
                            out=ot[oti][rows, cs], in0=acc[hh][rows, cs], scalar=ga[rows, j:j + 1], in1=rinv[ti][rows, :],
                            op0=ALU.mult, op1=ALU.mult), reads=[accB[hh], rinvB[ti], gaB], writes=[otB[oti]])
                rec.dma("sp", k.oT[j * 128:(j + 1) * 128, s0:s0 + S], ot[oti][:, 0:S], reads=[otB[oti]])
                rec.flush()
        rec.flush()


def phase_A3(k, l):
    nc, rec, cfg = k.nc, k.rec, k.cfg
    with ExitStack() as es:
        w = _sb(es, nc, "a3_w", [128, NCH, D_MODEL], BF16)
        wB = [Buf(f"a3_w{c}") for c in range(NCH)]
        for c in range(NCH):
            rec.dma("pool", w[:, c, :], k.w_out[l][c * 128:(c + 1) * 128, :], reads=[k.wbuf[f"w_out{l}"]], writes=[wB[c]])
        g2 = _sb(es, nc, "a3_g", [128, NCH], F32)
        gB = Buf("a3_g")
        k.epsT = _sb(es, nc, "a3_eps", [128, 1], F32)
        rec.op("dve", lambda e: e.memset(k.epsT[:], RMS_EPS), writes=[gB])
        rec.dma("sp", g2[:], k.norm2g[l], writes=[gB])
        wr = _sb(es, nc, "a3_wr", [128, NCH, N_EXPERTS], F32)
        rec.dma("sp", wr[:], k.w_router[l].rearrange("(c p) e -> p c e", p=128), writes=[gB])
        ob = [_sb(es, nc, f"a3_o{i}", [128, NCH, 512], BF16) for i in range(2)]
        obB = [Buf(f"a3_o{i}") for i in range(2)]
        xb = [_sb(es, nc, f"a3_x{i}", [128, NCH, 512], F32) for i in range(2)]
        xbB = [Buf(f"a3_x{i}") for i in range(2)]
        xn = [_sb(es, nc, f"a3_xn{i}", [128, NCH, 512], F32) for i in range(2)]
        xnB = [Buf(f"a3_xn{i}") for i in range(2)]
        sqb = _sb(es, nc, "a3_sq", [128, NCH, 512], BF16)
        sqB = Buf("a3_sq")
        tmp = _sb(es, nc, "a3_tmp", [128, 512], F32)
        tmpB = Buf("a3_tmp")
        rstd = _sb(es, nc, "a3_rstd", [128, 512], F32)
        rstdB = Buf("a3_rstd")
        h2f = _sb(es, nc, "a3_h2f", [128, NCH, 512], F32)
        h2fB = Buf("a3_h2f")
        h2b = [_sb(es, nc, f"a3_h2b{i}", [128, NCH, 512], BF16) for i in range(2)]
        h2bB = [Buf(f"a3_h2b{i}") for i in range(2)]
        ex = _sb(es, nc, "a3_ex", [16, 512], F32)
        exB = Buf("a3_ex")
        rs = _sb(es, nc, "a3_rs", [16, 512], F32)
        rsB = Buf("a3_rs")
        af = [_sb(es, nc, f"a3_af{i}", [16, 512], F32) for i in range(2)]
        afB = [Buf(f"a3_af{i}") for i in range(2)]
        ps_ss = _ps(es, nc, "a3_ss")
        ssB = Buf("a3_ss")
        ps = [_ps(es, nc, f"a3_ps{i}") for i in range(4)]
        psB = [Buf(f"a3_ps{i}") for i in range(4)]
        psr = _ps(es, nc, "a3_psr")
        psrB = Buf("a3_psr")
        pss = _ps(es, nc, "a3_pss")
        pssB = Buf("a3_pss")
        xTv = k.xT.rearrange("(c p) t -> p c t", p=128)
        oTv = k.oT.rearrange("(c p) t -> p c t", p=128)
        hTv = k.h2T.rearrange("(c p) t -> p c t", p=128)
        n = 0
        for b in range(cfg.NB):
            bi = b % 2
            blk = slice(b * 512, (b + 1) * 512)
            rec.dma("sp", ob[bi][:], oTv[:, :, blk], writes=[obB[bi]])
            rec.dma("sp", xb[bi][:], xTv[:, :, blk], writes=[xbB[bi]])
            for oc in range(NCH):
                pi = n % 4
                n += 1
                for kc in range(NCH):
                    rec.op("pe", lambda e, pi=pi, kc=kc, oc=oc, bi=bi: e.matmul(ps[pi][:, :], w[:, kc, oc * 128:(oc + 1) * 128],
                                                                               ob[bi][:, kc, :], start=(kc == 0), stop=(kc == NCH - 1)),
                           reads=[wB[kc], obB[bi]], writes=[psB[pi]])
                rec.op("dve", lambda e, pi=pi, oc=oc, bi=bi: e.tensor_tensor(out=xn[bi][:, oc, :], in0=xb[bi][:, oc, :], in1=ps[pi][:, :], op=ALU.add),
                       reads=[psB[pi], xbB[bi]], writes=[xnB[bi]])
            rec.dma("sp", xTv[:, :, blk], xn[bi][:], reads=[xnB[bi]])
            _rms_block(k, rec, xn[bi], xnB[bi], sqb, sqB, ps_ss, ssB, tmp, tmpB, rstd, rstdB)
            for c in range(NCH):
                rec.op("dve", lambda e, c=c, bi=bi: e.scalar_tensor_tensor(out=h2f[:, c, :], in0=xn[bi][:, c, :], scalar=g2[:, c:c + 1],
                                                                           in1=rstd[:], op0=ALU.mult, op1=ALU.mult),
                       reads=[xnB[bi], rstdB, gB], writes=[h2fB])
            rec.op("act", lambda e, bi=bi: e.activation(out=h2b[bi][:], in_=h2f[:], func=AF.Copy), reads=[h2fB], writes=[h2bB[bi]])
            rec.dma("sp", hTv[:, :, blk], h2b[bi][:], reads=[h2bB[bi]])
            for kc in range(NCH):
                rec.op("pe", lambda e, kc=kc: e.matmul(psr[0:16, :], wr[:, kc, :], h2f[:, kc, :], start=(kc == 0), stop=(kc == NCH - 1)),
                       reads=[gB, h2fB], writes=[psrB])
            rec.op("act", lambda e: e.activation(out=ex[:], in_=psr[0:16, :], func=AF.Exp), reads=[psrB], writes=[exB])
            rec.op("pe", lambda e: e.matmul(pss[0:16, :], k.onesF16[:], ex[:], start=True, stop=True), reads=[exB], writes=[pssB])
            rec.op("dve", lambda e: e.reciprocal(out=rs[:], in_=pss[0:16, :]), reads=[pssB], writes=[rsB])
            rec.op("dve", lambda e, bi=bi: e.tensor_tensor(out=af[bi][:], in0=ex[:], in1=rs[:], op=ALU.mult), reads=[exB, rsB], writes=[afB[bi]])
            rec.dma("sp", k.ag_in[:, blk], af[bi][:], reads=[afB[bi]])
            if k.aff_dbg is not None:
                rec.dma("sp", k.aff_dbg[:, blk], af[bi][:], reads=[afB[bi]])
        rec.flush()


N_BISECT = 26


def phase_R(k, l):
    nc, rec, cfg = k.nc, k.rec, k.cfg
    T, TP, TS = cfg.T, cfg.TP, cfg.TS
    groups = [list(range(N_CORES))]
    with ExitStack() as es:
        agB = Buf("r_ag")
        rec.cc(lambda e: e.collective_compute("AllGather", ALU.bypass, replica_groups=groups, ins=[k.ag_in], outs=[k.ag_out]),
               writes=[agB])
        A = _sb(es, nc, "r_A", [128, T], F32)
        AB = Buf("r_A")
        first = True
        for c0 in range(0, T, 2048):
            o = rec.dma("sp", A[:, c0:c0 + 2048], k.ag_out[:, c0:c0 + 2048], reads=[agB], writes=[AB] if first else [])
            if not first:
                AB.last_w.append(o)
            first = False
        junk = _sb(es, nc, "r_junk", [128, max(TP, TS)], BF16)
        jB = Buf("r_junk")
        G = _sb(es, nc, "r_G", [128, 2, 128], F32)
        GB = Buf("r_G")
        rec.dma("sp", G[:], k.gmat, writes=[GB])
        loc = _sb(es, nc, "r_loc", [16, T], F32)
        locB = Buf("r_loc")
        rec.dma("sp", loc[:], k.ag_in, writes=[locB])
        gm = _sb(es, nc, "r_gm", [16, T], F32)
        gmB = Buf("r_gm")
        sm = _sb(es, nc, "r_sm", [128, 16], F32)
        smB = [Buf(f"r_sm{i}") for i in range(16)]
        pst = _ps(es, nc, "r_ps")
        pstB = Buf("r_ps")
        grp = [(0, 0, TP, float(2 * (TP * N_CORES) // N_EXPERTS))]
        if TS:
            grp.append((1, TP, T, float(2 * (TS * (N_CORES // 2)) // N_EXPERTS)))
        for (gi, c0, c1, cap) in grp:
            LO, MID, CNT, GEW = 4 * gi, 4 * gi + 1, 4 * gi + 2, 4 * gi + 3
            col = lambda i: sm[:, i:i + 1]
            rec.op("dve", lambda e, LO=LO: e.memset(sm[:, LO:LO + 1], 0.0), writes=[smB[LO]])
            for it in range(N_BISECT):
                wdt = 2.0 ** (-(it + 1))
                rec.op("dve", lambda e, LO=LO, MID=MID, wdt=wdt: e.tensor_scalar(out=sm[:, MID:MID + 1], in0=sm[:, LO:LO + 1], scalar1=wdt,
                                                                              scalar2=None, op0=ALU.add),
                       reads=[smB[LO]], writes=[smB[MID]])
                rec.op("dve", lambda e, MID=MID, CNT=CNT, c0=c0, c1=c1: e.tensor_scalar(out=junk[:, 0:c1 - c0], in0=A[:, c0:c1], scalar1=sm[:, MID:MID + 1],
                                                                                      scalar2=0.0, op0=ALU.is_ge, op1=ALU.add,
                                                                                      accum_out=sm[:, CNT:CNT + 1]),
                       reads=[AB, smB[MID]], writes=[jB, smB[CNT]])
                rec.op("pe", lambda e, gi=gi, CNT=CNT: e.matmul(pst[:, 0:1], G[:, gi, :], sm[:, CNT:CNT + 1], start=True, stop=True),
                       reads=[GB, smB[CNT]], writes=[pstB])
                rec.op("dve", lambda e, GEW=GEW, cap=cap, wdt=wdt: e.tensor_scalar(out=sm[:, GEW:GEW + 1], in0=pst[:, 0:1], scalar1=cap, scalar2=wdt,
                                                                                  op0=ALU.is_ge, op1=ALU.mult),
                       reads=[pstB], writes=[smB[GEW]])
                rec.op("dve", lambda e, LO=LO, GEW=GEW: e.tensor_tensor(out=sm[:, LO:LO + 1], in0=sm[:, LO:LO + 1], in1=sm[:, GEW:GEW + 1], op=ALU.add),
                       reads=[smB[LO], smB[GEW]], writes=[smB[LO]])
            rec.op("dve", lambda e, LO=LO, c0=c0, c1=c1: e.scalar_tensor_tensor(out=gm[:, c0:c1], in0=loc[:, c0:c1], scalar=sm[0:16, LO:LO + 1],
                                                                               in1=loc[:, c0:c1], op0=ALU.is_ge, op1=ALU.mult),
                   reads=[locB, smB[LO]], writes=[gmB])
        rec.dma("sp", k.gmT, gm[:], reads=[gmB])
        if k.thr_dbg is not None:
            rec.dma("sp", k.thr_dbg, sm[:, 0:4], reads=smB[0:8])
        rec.flush()


def _cast_load(rec, q, dst, src, reads, wB):
    rec.dma(q, dst, src, reads=reads, writes=[wB])


def phase_F(k, l):
    nc, rec, cfg = k.nc, k.rec, k.cfg
    SBT = 512
    with ExitStack() as es:
        W = [[_sb(es, nc, f"f_w{i}_{m}", [128, NCH, D_MODEL], BF16) for m in range(3)] for i in range(2)]
        WB = [[[Buf(f"f_w{i}_{m}_{c}") for c in range(NCH)] for m in range(3)] for i in range(2)]
        h2 = _sb(es, nc, "f_h2", [128, NCH, SBT], BF16)
        h2B = Buf("f_h2")
        gm = _sb(es, nc, "f_gm", [16, SBT], F32)
        gmB = Buf("f_gm")
        yacc = _sb(es, nc, "f_y", [128, NCH, SBT], F32)
        yB = Buf("f_y")
        gmb = _sb(es, nc, "f_gmb", [128, 512], F32)
        gmbB = Buf("f_gmb")
        t1 = _sb(es, nc, "f_t1", [128, 512], F32)
        t1B = Buf("f_t1")
        t2 = _sb(es, nc, "f_t2", [128, 512], F32)
        t2B = Buf("f_t2")
        hid = _sb(es, nc, "f_hid", [128, NCH, 512], BF16)
        hidB = [Buf(f"f_hid{c}") for c in range(NCH)]
        xb = _sb(es, nc, "f_x", [128, NCH, 512], F32)
        xbB = Buf("f_x")
        pg = [_ps(es, nc, f"f_pg{i}") for i in range(2)]
        pgB = [Buf(f"f_pg{i}") for i in range(2)]
        pu = [_ps(es, nc, f"f_pu{i}") for i in range(2)]
        puB = [Buf(f"f_pu{i}") for i in range(2)]
        py = [_ps(es, nc, f"f_py{i}") for i in range(2)]
        pyB = [Buf(f"f_py{i}") for i in range(2)]
        pb = _ps(es, nc, "f_pb")
        pbB = Buf("f_pb")
        xTv = k.xT.rearrange("(c p) t -> p c t", p=128)
        hTv = k.h2T.rearrange("(c p) t -> p c t", p=128)
        wsrc = (k.w_gate[l], k.w_up[l], k.w_down[l])
        wnm = ("w_gate", "w_up", "w_down")
        n = 0
        pairs = [(sb0, ex) for sb0 in range(0, cfg.T, SBT) for ex in range(N_EXPERTS)]

        def load_w(pi_):
            ex_ = pairs[pi_][1]
            wi_ = pi_ % 2
            for m in range(3):
                for c in range(NCH):
                    r0 = ex_ * D_MODEL + c * 128
                    rec.dma("pool", W[wi_][m][:, c, :], wsrc[m][r0:r0 + 128, :], reads=[k.wbuf[f"{wnm[m]}{l}"]], writes=[WB[wi_][m][c]])

        load_w(0)
        for pidx, (sb0, ex) in enumerate(pairs):
            if ex == 0:
                rec.dma("sp", h2[:], hTv[:, :, sb0:sb0 + SBT], writes=[h2B])
                rec.dma("sp", gm[:], k.gmT[:, sb0:sb0 + SBT], writes=[gmB])
            if pidx + 1 < len(pairs):
                load_w(pidx + 1)
            wi = pidx % 2
            if True:
                wg, wu, wd = W[wi]
                wgB, wuB, wdB = WB[wi]
                for hf in range(SBT // 512):
                    cs = slice(hf * 512, (hf + 1) * 512)
                    rec.op("pe", lambda e, ex=ex, cs=cs: e.matmul(pb[:, :], k.selF[:, ex, :], gm[:, cs], start=True, stop=True),
                           reads=[gmB], writes=[pbB])
                    rec.op("act", lambda e: e.activation(out=gmb[:], in_=pb[:, :], func=AF.Copy), reads=[pbB], writes=[gmbB])
                    for fc in range(NCH):
                        pi = n % 2
                        n += 1
                        for kc in range(NCH):
                            rec.op("pe", lambda e, pi=pi, kc=kc, fc=fc, cs=cs, wg=wg: e.matmul(pg[pi][:, :], wg[:, kc, fc * 128:(fc + 1) * 128], h2[:, kc, cs],
                                                                                           start=(kc == 0), stop=(kc == NCH - 1)),
                                   reads=[wgB[kc], h2B], writes=[pgB[pi]])
                        for kc in range(NCH):
                            rec.op("pe", lambda e, pi=pi, kc=kc, fc=fc, cs=cs, wu=wu: e.matmul(pu[pi][:, :], wu[:, kc, fc * 128:(fc + 1) * 128], h2[:, kc, cs],
                                                                                           start=(kc == 0), stop=(kc == NCH - 1)),
                                   reads=[wuB[kc], h2B], writes=[puB[pi]])
                        rec.op("act", lambda e, pi=pi: e.activation(out=t1[:], in_=pg[pi][:, :], func=AF.Sigmoid), reads=[pgB[pi]], writes=[t1B])
                        rec.op("dve", lambda e, pi=pi: e.tensor_tensor(out=t1[:], in0=t1[:], in1=pg[pi][:, :], op=ALU.mult),
                               reads=[pgB[pi], t1B], writes=[t1B])
                        rec.op("dve", lambda e, pi=pi: e.tensor_tensor(out=t2[:], in0=gmb[:], in1=pu[pi][:, :], op=ALU.mult),
                               reads=[puB[pi], gmbB], writes=[t2B])
                        rec.op("dve", lambda e, fc=fc: e.tensor_tensor(out=hid[:, fc, :], in0=t1[:], in1=t2[:], op=ALU.mult),
                               reads=[t1B, t2B], writes=[hidB[fc]])
                    for dc in range(NCH):
                        pi = n % 2
                        n += 1
                        for fc in range(NCH):
                            rec.op("pe", lambda e, pi=pi, fc=fc, dc=dc, wd=wd: e.matmul(py[pi][:, :], wd[:, fc, dc * 128:(dc + 1) * 128], hid[:, fc, :],
                                                                                    start=(fc == 0), stop=(fc == NCH - 1)),
                                   reads=[wdB[fc], hidB[fc]], writes=[pyB[pi]])
                        if ex == 0:
                            rec.op("act", lambda e, pi=pi, dc=dc, cs=cs: e.activation(out=yacc[:, dc, cs], in_=py[pi][:, :], func=AF.Copy),
                                   reads=[pyB[pi]], writes=[yB])
                        else:
                            rec.op("dve", lambda e, pi=pi, dc=dc, cs=cs: e.tensor_tensor(out=yacc[:, dc, cs], in0=yacc[:, dc, cs], in1=py[pi][:, :], op=ALU.add),
                                   reads=[pyB[pi], yB], writes=[yB])
            for hf in (range(SBT // 512) if ex == N_EXPERTS - 1 else ()):
                cs = slice(hf * 512, (hf + 1) * 512)
                gs = slice(sb0 + hf * 512, sb0 + (hf + 1) * 512)
                rec.dma("sp", xb[:], xTv[:, :, gs], writes=[xbB])
                rec.op("dve", lambda e, cs=cs: e.tensor_tensor(out=xb[:], in0=xb[:], in1=yacc[:, :, cs], op=ALU.add),
                       reads=[xbB, yB], writes=[xbB])
                rec.dma("sp", xTv[:, :, gs], xb[:], reads=[xbB])
        rec.flush()


def phase_Z(k):
    nc, rec, cfg = k.nc, k.rec, k.cfg
    with ExitStack() as es:
        gF = _sb(es, nc, "z_g", [128, NCH], F32)
        gB = Buf("z_g")
        k.epsT = _sb(es, nc, "z_eps", [128, 1], F32)
        rec.op("dve", lambda e: e.memset(k.epsT[:], RMS_EPS), writes=[gB])
        rec.dma("sp", gF[:], k.finalg, writes=[gB])
        xb = [_sb(es, nc, f"z_x{i}", [128, NCH, 512], F32) for i in range(2)]
        xbB = [Buf(f"z_x{i}") for i in range(2)]
        sqb = _sb(es, nc, "z_sq", [128, NCH, 512], BF16)
        sqB = Buf("z_sq")
        tmp = _sb(es, nc, "z_tmp", [128, 512], F32)
        tmpB = Buf("z_tmp")
        rstd = _sb(es, nc, "z_rstd", [128, 512], F32)
        rstdB = Buf("z_rstd")
        y = [_sb(es, nc, f"z_y{i}", [128, NCH, 512], F32) for i in range(2)]
        yB = [Buf(f"z_y{i}") for i in range(2)]
        yt = [_sb(es, nc, f"z_yt{i}", [128, D_MODEL], F32) for i in range(3)]
        ytB = [Buf(f"z_yt{i}") for i in range(3)]
        ps_ss = _ps(es, nc, "z_ss")
        ssB = Buf("z_ss")
        ps = [_ps(es, nc, f"z_ps{i}") for i in range(4)]
        psB = [Buf(f"z_ps{i}") for i in range(4)]
        xTv = k.xT.rearrange("(c p) t -> p c t", p=128)
        n = 0
        m = 0
        for b in range(cfg.NB):
            bi = b % 2
            blk = slice(b * 512, (b + 1) * 512)
            rec.dma("sp", xb[bi][:], xTv[:, :, blk], writes=[xbB[bi]])
            _rms_block(k, rec, xb[bi], xbB[bi], sqb, sqB, ps_ss, ssB, tmp, tmpB, rstd, rstdB)
            for c in range(NCH):
                rec.op("dve", lambda e, c=c, bi=bi: e.scalar_tensor_tensor(out=y[bi][:, c, :], in0=xb[bi][:, c, :], scalar=gF[:, c:c + 1],
                                                                           in1=rstd[:], op0=ALU.mult, op1=ALU.mult),
                       reads=[xbB[bi], rstdB, gB], writes=[yB[bi]])
            for s in range(4):
                yi = m % 3
                m += 1
                for half in range(2):
                    pi = n % 4
                    n += 1
                    for cc in range(4):
                        c = half * 4 + cc
                        rec.op("pe", lambda e, pi=pi, cc=cc, c=c, s=s, bi=bi: e.transpose(ps[pi][:, cc * 128:(cc + 1) * 128],
                                                                                         y[bi][:, c, s * 128:(s + 1) * 128], k.identF[:]),
                               reads=[yB[bi]], writes=[psB[pi]])
                    _evac(rec, half, yt[yi][:, half * 512:(half + 1) * 512], ps[pi][:, :], [psB[pi]], [ytB[yi]])
                r0 = (b * 4 + s) * 128
                rec.dma("sp", k.yout[r0:r0 + 128, :], yt[yi][:], reads=[ytB[yi]])
        rec.flush()


def _pc(v):
    v = np.asarray(v, np.float32)
    return np.ascontiguousarray(np.swapaxes(v.reshape(v.shape[:-1] + (NCH, 128)), -1, -2))


_NA_IDX = None


def _na_bias_layout(rpb):
    global _NA_IDX
    if _NA_IDX is None:
        _NA_IDX = na_bias_index()
    ri, ci = _NA_IDX
    valid = ri >= 0
    g = np.asarray(rpb, np.float32)[:, :, np.where(valid, ri, 0), np.where(valid, ci, 0)]
    g = np.where(valid[None, None], g, np.float32(-30000.0)).astype(np.float32)
    L = g.shape[0]
    return np.ascontiguousarray(g.reshape(L * N_HEADS_NA * len(NA_CLASSES) * 128, 5 * 128))


def make_in_maps(cfg, x_prompt, x_sample, norm1, w_in, rpb, gain_na, gain_dil, w_out, norm2, w_router, w_gate, w_up,
                 w_down, final_norm):
    import ml_dtypes
    L = cfg.depth
    f32 = lambda a: np.asarray(a, np.float32)
    norm1, w_in, rpb, gain_na, gain_dil, w_out, norm2, w_router, w_gate, w_up, w_down = [
        f32(a)[:L] for a in (norm1, w_in, rpb, gain_na, gain_dil, w_out, norm2, w_router, w_gate, w_up, w_down)]
    nab = _na_bias_layout(rpb)
    nab_rows = nab.shape[0] // N_CORES
    dil = np.ascontiguousarray(dil_mask_const().reshape(128, -1)).astype(ml_dtypes.bfloat16)
    gattn = _pc(np.concatenate([gain_na, gain_dil], axis=-1))
    p = np.arange(128)
    same = (p[:, None] % 16) == (p[None, :] % 16)
    gmat = np.stack([same, same & (p[:, None] < 64)], axis=1).astype(np.float32)
    common = {
        "norm1g": _pc(norm1), "dilmask": dil, "gattn": gattn, "norm2g": _pc(norm2),
        "w_router": np.ascontiguousarray(w_router), "finalg": _pc(f32(final_norm)), "gmat": np.ascontiguousarray(gmat),
    }
    xp = f32(x_prompt)
    xs = f32(x_sample)
    maps = []
    for c in range(N_CORES):
        parts = [xp[cfg.NP * c:cfg.NP * (c + 1)].reshape(-1, D_MODEL)]
        if cfg.TS:
            parts.append(xs[c % xs.shape[0]].reshape(-1, D_MODEL))
        m = dict(common)
        m["xin"] = np.ascontiguousarray(np.concatenate(parts, axis=0))
        m["w_in_s"] = np.ascontiguousarray(w_in[:, c * 128:(c + 1) * 128, :].reshape(L * 128, -1))
        m["w_out_s"] = np.ascontiguousarray(w_out[:, c * 128:(c + 1) * 128, :].reshape(L * 128, -1))
        m["w_gate_s"] = np.ascontiguousarray(w_gate[:, 2 * c:2 * c + 2].reshape(L * 2 * D_MODEL, D_MODEL))
        m["w_up_s"] = np.ascontiguousarray(w_up[:, 2 * c:2 * c + 2].reshape(L * 2 * D_MODEL, D_MODEL))
        m["w_down_s"] = np.ascontiguousarray(w_down[:, 2 * c:2 * c + 2].reshape(L * 2 * D_MODEL, D_MODEL))
        m["nabias_s"] = np.ascontiguousarray(nab[c * nab_rows:(c + 1) * nab_rows])
        maps.append(m)
    return maps


_PROG = {}


def kernel(x_prompt, x_sample, norm1, w_in, rpb, gain_na, gain_dil, w_out, norm2, w_router, w_gate, w_up, w_down,
           final_norm):
    cfg = Cfg(n_prompt=4, depth=4, with_sample=True)
    maps = make_in_maps(cfg, x_prompt, x_sample, norm1, w_in, rpb, gain_na, gain_dil, w_out, norm2, w_router, w_gate,
                        w_up, w_down, final_norm)
    if "nc" not in _PROG:
        _PROG["nc"] = build_program(cfg)
    res = run_bass_kernel_spmd(_PROG["nc"], maps, core_ids=list(range(N_CORES)))
    outs = [np.asarray(r["yout"], np.float32) for r in res.results]
    yp = np.stack([o[:cfg.TP].reshape(cfg.NP, SEQ_P, D_MODEL) for o in outs]).reshape(-1, SEQ_P, D_MODEL)
    ys = np.stack([outs[c][cfg.TP:].reshape(SEQ_S, D_MODEL) for c in range(4)])
    return (yp, ys)
```
